# Optimizing a Trainium2 kernel written in Bass

```python
import jax, jax.numpy as jnp
from jax import lax
import numpy as np

D_MODEL = 2048
BATCH = 4
SEQ = 8192
DEPTH = 2
DEC_BATCH = 8
DEC_SEQ = 16
PAST_LEN = 2048

CHUNK = 64
Q_BLOCK = 128
N_HEADS = D_MODEL // 128
Q_LORA = 512
KV_LORA = 512
NOPE_DIM = 128
ROPE_DIM = 64
V_DIM = 128
QK_DIM = NOPE_DIM + ROPE_DIM
ROPE_THETA = 10000.0
D_MIX = N_HEADS * V_DIM
D_LRU = D_MODEL
LRU_BLOCKS = D_LRU // 128
LRU_BW = D_LRU // LRU_BLOCKS
CONV_W = 4
LRU_C = 8.0
D_FF = 5504
N_EXPERTS = 8
TOP_K = 2
D_FF_EXPERT = 7168
N_DENSE = (DEPTH + 1) // 2
N_MOE = DEPTH // 2
DN_ALPHA = (2 * DEPTH) ** 0.25
DN_BETA = (8 * DEPTH) ** -0.25
LN_EPS = 1e-5
RMS_EPS = 1e-6
IN_SPLITS = (Q_LORA, KV_LORA, ROPE_DIM, D_LRU, D_LRU, D_MODEL, D_MODEL)
D_IN = sum(IN_SPLITS)
IN_OFFSETS = tuple(int(v) for v in np.cumsum(IN_SPLITS)[:-1])

kernel_name = 'hybrid_rglru_mla_streaming_step'


def layer_norm(x, g, b):
    xf = x.astype(jnp.float32)
    mu = jnp.mean(xf, -1, keepdims=True)
    var = jnp.mean(jnp.square(xf - mu), -1, keepdims=True)
    return ((xf - mu) * lax.rsqrt(var + LN_EPS) * g + b).astype(x.dtype)


def rms_norm(x, g):
    xf = x.astype(jnp.float32)
    return (xf * lax.rsqrt(jnp.mean(xf * xf, -1, keepdims=True) + RMS_EPS) * g).astype(x.dtype)


def rope(x, pos):
    half = ROPE_DIM // 2
    inv = ROPE_THETA ** (-jnp.arange(half, dtype=jnp.float32) / half)
    ang = pos.astype(jnp.float32)[:, None] * inv[None, :]
    cos = jnp.cos(ang)[:, None, :]
    sin = jnp.sin(ang)[:, None, :]
    xf = x.astype(jnp.float32)
    x1, x2 = xf[..., :half], xf[..., half:]
    return jnp.concatenate([x1 * cos - x2 * sin, x1 * sin + x2 * cos], -1).astype(x.dtype)


def attend_block(q_nope, q_rope, q_pos, k_nope, k_rope, v, k_pos):
    s = (jnp.einsum('bqhn,bkhn->bhqk', q_nope, k_nope)
         + jnp.einsum('bqhr,bkr->bhqk', q_rope, k_rope)).astype(jnp.float32) * (QK_DIM ** -0.5)
    mask = (k_pos[None, :] // CHUNK) <= (q_pos[:, None] // CHUNK)
    p = jax.nn.softmax(jnp.where(mask, s, -jnp.inf), axis=-1)
    return jnp.einsum('bhqk,bkhv->bqhv', p.astype(v.dtype), v)


def mla_attention(q_nope, q_rope, q_pos, k_nope, k_rope, v, k_pos):
    B, S = q_nope.shape[:2]
    if S % Q_BLOCK:
        return attend_block(q_nope, q_rope, q_pos, k_nope, k_rope, v, k_pos)
    nb = S // Q_BLOCK
    qn = q_nope.reshape(B, nb, Q_BLOCK, N_HEADS, NOPE_DIM).swapaxes(0, 1)
    qr = q_rope.reshape(B, nb, Q_BLOCK, N_HEADS, ROPE_DIM).swapaxes(0, 1)
    qp = q_pos.reshape(nb, Q_BLOCK)

    def blk(args):
        a, r, p = args
        return attend_block(a, r, p, k_nope, k_rope, v, k_pos)

    out = lax.map(blk, (qn, qr, qp))
    return out.swapaxes(0, 1).reshape(B, S, N_HEADS, V_DIM)


def causal_conv(u, buf, w, b):
    S = u.shape[1]
    uc = jnp.concatenate([buf, u], axis=1)
    y = b
    for j in range(CONV_W):
        y = y + uc[:, j:j + S] * w[j]
    return y, uc[:, -(CONV_W - 1):]


def rg_lru(u, h0, w_a, b_a, w_x, b_x, lam):
    B, S, W = u.shape
    ub = u.reshape(B, S, LRU_BLOCKS, LRU_BW)
    r = jax.nn.sigmoid(jnp.einsum('bsnc,ncd->bsnd', ub, w_a) + b_a).reshape(B, S, W)
    i = jax.nn.sigmoid(jnp.einsum('bsnc,ncd->bsnd', ub, w_x) + b_x).reshape(B, S, W)
    log_a = (-LRU_C * r.astype(jnp.float32)) * jax.nn.softplus(-lam.astype(jnp.float32))
    a = jnp.exp(log_a)
    bt = jnp.sqrt(-jnp.expm1(2.0 * log_a)) * (i * u).astype(jnp.float32)
    bt = bt.at[:, 0].add(a[:, 0] * h0.astype(jnp.float32))

    def combine(lhs, rhs):
        a1, b1 = lhs
        a2, b2 = rhs
        return a1 * a2, a2 * b1 + b2

    _, h = lax.associative_scan(combine, (a, bt), axis=1)
    h = h.astype(u.dtype)
    return h, h[:, -1]


def temporal_mix(x, pos, past_lat, past_kr, h0, conv_buf, w_in, q_norm_g, w_uq, kv_norm_g, w_uk, w_uv,
                 conv_w, conv_b, w_gate_a, b_gate_a, w_gate_x, b_gate_x, lru_lambda, w_o):
    B, S, _ = x.shape
    proj = jnp.einsum('bsd,de->bse', x, w_in)
    c_q, c_kv, k_r, u, lru_gate, g_a, g_b = jnp.split(proj, IN_OFFSETS, axis=-1)
    q = jnp.einsum('bsc,ce->bse', rms_norm(c_q, q_norm_g), w_uq).reshape(B, S, N_HEADS, QK_DIM)
    q_nope = q[..., :NOPE_DIM]
    q_rope = rope(q[..., NOPE_DIM:], pos)
    lat = rms_norm(c_kv, kv_norm_g)
    k_rope = rope(k_r[:, :, None, :], pos)[:, :, 0]
    if past_lat is None:
        lat_all, kr_all, k_pos = lat, k_rope, pos
    else:
        lat_all = jnp.concatenate([past_lat, lat], axis=1)
        kr_all = jnp.concatenate([past_kr, k_rope], axis=1)
        k_pos = jnp.concatenate([jnp.arange(past_lat.shape[1], dtype=pos.dtype), pos])
    K = lat_all.shape[1]
    k_nope = jnp.einsum('bkc,ce->bke', lat_all, w_uk).reshape(B, K, N_HEADS, NOPE_DIM)
    v = jnp.einsum('bkc,ce->bke', lat_all, w_uv).reshape(B, K, N_HEADS, V_DIM)
    attn = mla_attention(q_nope, q_rope, pos, k_nope, kr_all, v, k_pos).reshape(B, S, D_MIX)
    u_c, new_buf = causal_conv(u, conv_buf, conv_w, conv_b)
    h, h_last = rg_lru(u_c, h0, w_gate_a, b_gate_a, w_gate_x, b_gate_x, lru_lambda)
    lru_out = h * jax.nn.gelu(lru_gate)
    merged = jax.nn.sigmoid(g_a) * lru_out + jax.nn.sigmoid(g_b) * attn
    out = jnp.einsum('bse,ed->bsd', merged, w_o)
    return out, lat, k_rope, h_last, new_buf


def swiglu(x, w1, w3, w2):
    return jnp.matmul(jax.nn.silu(jnp.matmul(x, w1)) * jnp.matmul(x, w3), w2)


def moe(x, w_router, w1, w3, w2):
    logits = jnp.einsum('bsd,de->bse', x, w_router).astype(jnp.float32)
    top_v, top_i = lax.top_k(logits, TOP_K)
    gates = jax.nn.softmax(top_v, axis=-1)
    comb = jnp.sum(jax.nn.one_hot(top_i, N_EXPERTS, dtype=jnp.float32) * gates[..., None], axis=-2)
    y = jnp.zeros_like(x)
    for e in range(N_EXPERTS):
        y = y + comb[..., e:e + 1].astype(x.dtype) * swiglu(x, w1[e], w3[e], w2[e])
    return y


def setup_inputs(seed: int = 0) -> dict:
    key = jax.random.key(seed)
    ks = iter(jax.random.split(key, 40))

    def nrm(shape, scale):
        return jax.random.normal(next(ks), shape, jnp.float32) * scale

    a8 = jax.random.uniform(next(ks), (DEPTH, D_LRU), jnp.float32, 0.9, 0.999)
    a = a8 ** (1.0 / LRU_C)
    lam = jnp.log(a) - jnp.log1p(-a)
    return {
        'x_prompt': nrm((BATCH, SEQ, D_MODEL), 1.0),
        'x_sample': nrm((DEC_BATCH, DEC_SEQ, D_MODEL), 1.0),
        'cache_kv_latent': nrm((DEPTH, DEC_BATCH, PAST_LEN, KV_LORA), 1.0),
        'cache_k_rope': nrm((DEPTH, DEC_BATCH, PAST_LEN, ROPE_DIM), 1.0),
        'state_lru': nrm((DEPTH, DEC_BATCH, D_LRU), 0.5),
        'state_conv': nrm((DEPTH, DEC_BATCH, CONV_W - 1, D_LRU), 1.0),
        'w_in': nrm((DEPTH, D_MODEL, D_IN), D_MODEL ** -0.5),
        'q_norm_g': 1.0 + nrm((DEPTH, Q_LORA), 0.02),
        'w_uq': nrm((DEPTH, Q_LORA, N_HEADS * QK_DIM), Q_LORA ** -0.5),
        'kv_norm_g': 1.0 + nrm((DEPTH, KV_LORA), 0.02),
        'w_uk': nrm((DEPTH, KV_LORA, N_HEADS * NOPE_DIM), KV_LORA ** -0.5),
        'w_uv': nrm((DEPTH, KV_LORA, N_HEADS * V_DIM), KV_LORA ** -0.5 * DN_BETA),
        'conv_w': nrm((DEPTH, CONV_W, D_LRU), CONV_W ** -0.5),
        'conv_b': nrm((DEPTH, D_LRU), 0.02),
        'w_gate_a': nrm((DEPTH, LRU_BLOCKS, LRU_BW, LRU_BW), LRU_BW ** -0.5),
        'b_gate_a': nrm((DEPTH, LRU_BLOCKS, LRU_BW), 0.02),
        'w_gate_x': nrm((DEPTH, LRU_BLOCKS, LRU_BW, LRU_BW), LRU_BW ** -0.5),
        'b_gate_x': nrm((DEPTH, LRU_BLOCKS, LRU_BW), 0.02),
        'lru_lambda': lam,
        'w_o': nrm((DEPTH, D_MIX, D_MODEL), D_MIX ** -0.5 * DN_BETA),
        'ln1_g': 1.0 + nrm((DEPTH, D_MODEL), 0.02),
        'ln1_b': nrm((DEPTH, D_MODEL), 0.02),
        'ln2_g': 1.0 + nrm((DEPTH, D_MODEL), 0.02),
        'ln2_b': nrm((DEPTH, D_MODEL), 0.02),
        'ffn_w1': nrm((N_DENSE, D_MODEL, D_FF), D_MODEL ** -0.5),
        'ffn_w3': nrm((N_DENSE, D_MODEL, D_FF), D_MODEL ** -0.5),
        'ffn_w2': nrm((N_DENSE, D_FF, D_MODEL), D_FF ** -0.5 * DN_BETA),
        'router_w': nrm((N_MOE, D_MODEL, N_EXPERTS), D_MODEL ** -0.5),
        'moe_w1': nrm((N_MOE, N_EXPERTS, D_MODEL, D_FF_EXPERT), D_MODEL ** -0.5),
        'moe_w3': nrm((N_MOE, N_EXPERTS, D_MODEL, D_FF_EXPERT), D_MODEL ** -0.5),
        'moe_w2': nrm((N_MOE, N_EXPERTS, D_FF_EXPERT, D_MODEL), D_FF_EXPERT ** -0.5 * DN_BETA),
    }


def reference(x_prompt, x_sample, cache_kv_latent, cache_k_rope, state_lru, state_conv,
              w_in, q_norm_g, w_uq, kv_norm_g, w_uk, w_uv, conv_w, conv_b,
              w_gate_a, b_gate_a, w_gate_x, b_gate_x, lru_lambda, w_o,
              ln1_g, ln1_b, ln2_g, ln2_b, ffn_w1, ffn_w3, ffn_w2,
              router_w, moe_w1, moe_w3, moe_w2):
    mix_w = (w_in, q_norm_g, w_uq, kv_norm_g, w_uk, w_uv, conv_w, conv_b,
             w_gate_a, b_gate_a, w_gate_x, b_gate_x, lru_lambda, w_o)

    def trunk(x, pos, past_lat, past_kr, h0, conv0):
        B = x.shape[0]
        lats, krs, hs, bufs = [], [], [], []
        for l in range(DEPTH):
            if past_lat is None:
                pl, pk = None, None
                h_in = jnp.zeros((B, D_LRU), x.dtype)
                c_in = jnp.zeros((B, CONV_W - 1, D_LRU), x.dtype)
            else:
                pl, pk, h_in, c_in = past_lat[l], past_kr[l], h0[l], conv0[l]
            mix, lat, kr, h_last, buf = temporal_mix(x, pos, pl, pk, h_in, c_in, *[w[l] for w in mix_w])
            x = layer_norm(DN_ALPHA * x + mix, ln1_g[l], ln1_b[l])
            if l % 2 == 0:
                f = swiglu(x, ffn_w1[l // 2], ffn_w3[l // 2], ffn_w2[l // 2])
            else:
                f = moe(x, router_w[l // 2], moe_w1[l // 2], moe_w3[l // 2], moe_w2[l // 2])
            x = layer_norm(DN_ALPHA * x + f, ln2_g[l], ln2_b[l])
            lats.append(lat)
            krs.append(kr)
            hs.append(h_last)
            bufs.append(buf)
        return x, jnp.stack(lats), jnp.stack(krs), jnp.stack(hs), jnp.stack(bufs)

    pos_p = jnp.arange(x_prompt.shape[1], dtype=jnp.int32)
    pos_s = cache_kv_latent.shape[2] + jnp.arange(x_sample.shape[1], dtype=jnp.int32)
    y_prompt, p_lat, p_kr, p_lru, p_conv = trunk(x_prompt, pos_p, None, None, None, None)
    y_sample, s_lat, s_kr, s_lru, s_conv = trunk(x_sample, pos_s, cache_kv_latent, cache_k_rope, state_lru, state_conv)
    return (y_prompt, y_sample, p_lat, p_kr, p_lru, p_conv, s_lat, s_kr, s_lru, s_conv)
```

```python
import numpy as np
from contextlib import ExitStack
import concourse.bass as bass
import concourse.mybir as mybir
from concourse.bass_utils import run_bass_kernel_spmd

F32 = mybir.dt.float32
BF16 = mybir.dt.bfloat16
AF = mybir.ActivationFunctionType
ALU = mybir.AluOpType


class Cfg:
    D = 2048
    QL = 512
    KVL = 512
    RD = 64
    H = 16
    DFF = 5504
    DFFE = 7168
    NE = 8
    S = 8192
    TW = 512
    PAST = 2048
    DS = 16
    NPAIR = 4
    NSAMP = 8
    NCORES = 8


NV = 200


def vec_off():
    o = {}
    c = 0
    for name, n in (("qg", 4), ("kvg", 4), ("cw", 64), ("cb", 16), ("ba", 16), ("bx", 16), ("lam", 16),
                    ("l1g", 16), ("l1b", 16), ("l2g", 16), ("l2b", 16)):
        o[name] = c
        c += n
    assert c == NV
    return o


VO = vec_off()


class T:
    def __init__(self, name):
        self.name = name
        self.lw = {}
        self.rd = {}
        self.sem = None
        self.cnt = 0


class Eng:
    def __init__(self, name, h, sem):
        self.name = name
        self.h = h
        self.sem = sem
        self.cnt = 0
        self.waited = {}


class _Stop(Exception):
    pass


def build(cfg, kstop=None):
    D, QL, KVL, RD, H = cfg.D, cfg.QL, cfg.KVL, cfg.RD, cfg.H
    KC = D // 128
    DFF, DFFE, NE = cfg.DFF, cfg.DFFE, cfg.NE
    S, TWP, PAST, DS = cfg.S, cfg.TW, cfg.PAST, cfg.DS
    NT = S // TWP
    NH1 = NT // 2
    DINP = QL + KVL + 128 + KC * 512
    NFC, NFCE = DFF // 128, DFFE // 128
    ALPHA = float(4 ** 0.25)
    SCALE = float(192 ** -0.5)

    nc = bass.Bass("TRN2", target_bir_lowering=False)
    es = ExitStack()

    def din(name, shape, dt=F32):
        return nc.dram_tensor(name, list(shape), dt, kind="ExternalInput").ap()

    def dout(name, shape, dt=F32):
        return nc.dram_tensor(name, list(shape), dt, kind="ExternalOutput").ap()

    def dscr(name, shape, dt):
        return nc.dram_tensor(name, list(shape), dt, kind="Internal").ap()

    x_in = din("x", [S, D])
    xs_in = din("xs", [DS, D])
    clat = din("clat", [2, PAST, KVL])
    ckr = din("ckr", [2, PAST, RD])
    slru = din("slru", [2, 128, KC])
    sconv = din("sconv", [2, 128, KC, 3])
    w_in_f = din("w_in", [2, D, DINP])
    w_uq_f = din("w_uq", [2, QL, H * 192])
    w_uk_f = din("w_uk", [2, KVL, D])
    w_uv_f = din("w_uv", [2, KVL, D])
    w_ga_f = din("w_ga", [2, KC * 128, 128])
    w_gx_f = din("w_gx", [2, KC * 128, 128])
    w_o_f = din("w_o", [2, D, D])
    w13_f = din("w13", [D, 2 * DFF])
    w2_f = din("w2", [DFF, D])
    wr_f = din("wr", [D, NE])
    m13_f = din("m13", [NE, D, 2 * DFFE])
    m2_f = din("m2", [NE, DFFE, D])
    vecs_in = din("vecs", [128, 2 * NV])
    tmask_in = din("tmask", [128, NT])
    kbias_in = din("kbias", [128, 4 * NT])
    rope_in = din("rope", [64, 2, S])
    ropes_in = din("ropes", [64, 2, DS])
    ident_in = din("ident", [128, 128])
    rm_in = din("rm", [64, 64])
    sel_in = din("sel", [NE, NE, 128])

    y_o = dout("y", [NH1 * TWP, D])
    lat_o = dout("lat_o", [2, S, KVL])
    kr_o = dout("kr_o", [2, S, RD])
    lru_o = dout("lru_o", [2, 128, KC])
    conv_o = dout("conv_o", [2, 128, KC, 3])
    ys_o = dout("ys", [DS, D])
    slat_o = dout("slat_o", [2, DS, KVL])
    skr_o = dout("skr_o", [2, DS, RD])
    slru_o = dout("slru_o", [2, 128, KC])
    sconv_o = dout("sconv_o", [2, 128, KC, 3])

    w_in_b = dscr("w_in_b", [2, D, DINP], BF16)
    w_uq_b = dscr("w_uq_b", [2, QL, H * 192], BF16)
    w_uk_b = dscr("w_uk_b", [2, KVL, D], BF16)
    w_uv_b = dscr("w_uv_b", [2, KVL, D], BF16)
    w_ga_b = dscr("w_ga_b", [2, KC * 128, 128], BF16)
    w_gx_b = dscr("w_gx_b", [2, KC * 128, 128], BF16)
    w_o_b = dscr("w_o_b", [2, D, D], BF16)
    w13_b = dscr("w13_b", [D, 2 * DFF], BF16)
    w2_b = dscr("w2_b", [DFF, D], BF16)
    m13_b = [dscr("m13_b%d" % e, [D, 2 * DFFE], BF16) for e in range(NE)]
    m2_b = [dscr("m2_b%d" % e, [DFFE, D], BF16) for e in range(NE)]
    x1f = dscr("x1f", [NT, 128, KC, TWP], F32)
    x1b = dscr("x1b", [NT, 128, KC, TWP], BF16)
    xs1f = dscr("xs1f", [128, KC, DS], F32)
    xs1b = dscr("xs1b", [128, KC, DS], BF16)
    Kn_d = dscr("Kn_d", [H, 128, S], BF16)
    Kr_d = dscr("Kr_d", [64, S], BF16)
    V_d = dscr("V_d", [H, S, 128], BF16)
    SK = PAST + DS
    Kns_d = dscr("Kns_d", [H, 128, SK], BF16)
    Krs_d = dscr("Krs_d", [64, SK], BF16)
    Vs_d = dscr("Vs_d", [H, SK, 128], BF16)

    def newsem(name):
        return es.enter_context(nc.semaphore(name))

    PE = Eng("pe", nc.tensor, newsem("s_pe"))
    ACT = Eng("act", nc.scalar, newsem("s_act"))
    DVE = Eng("dve", nc.vector, newsem("s_dve"))
    POOL = Eng("pool", nc.gpsimd, newsem("s_pool"))
    SP = Eng("sp", nc.sync, newsem("s_sp"))
    import os as _os
    _kdbg = int(_os.environ.get("KDBG", "1000"))

    def ck2(n):
        if n >= _kdbg:
            raise _Stop()
    semcount = [0]
    all_dma = {}

    def tile_sem(t):
        if t.sem is None:
            t.sem = newsem("d_" + t.name)
            semcount[0] += 1
        return t.sem

    def emit_wait(E, sem, val):
        key = id(sem)
        if E.waited.get(key, 0) >= val:
            return
        E.h.wait_ge(sem, val)
        E.waited[key] = val

    def sync_deps(E, reads, writes, skip_self=False):
        deps = {}

        def add(d):
            for k, (sem, val) in d.items():
                if skip_self and sem is E.sem:
                    continue
                if k not in deps or deps[k][1] < val:
                    deps[k] = (sem, val)
        for r in reads:
            add(r.lw)
        for w in writes:
            add(w.lw)
            add(w.rd)
        for sem, val in deps.values():
            emit_wait(E, sem, val)

    def op(E, fn, reads=(), writes=(), inc=True, multi=False):
        sync_deps(E, reads, writes, skip_self=(E is PE))
        ins = fn()
        if inc:
            E.cnt += 1
            ins.then_inc(E.sem, 1)
            tag = (E.sem, E.cnt)
        else:
            tag = (E.sem, E.cnt + 1)
        k = id(E.sem)
        for r in reads:
            r.rd[k] = tag
        for w in writes:
            if multi:
                w.lw[k] = tag
            else:
                w.lw = {k: tag}
                w.rd = {}
        return ins

    def dma(Q, out_ap, in_ap, src, dst, semtile, multi=False, noncontig=False):
        sync_deps(Q, [src], [dst])
        sem = tile_sem(semtile)
        semtile.cnt += 1
        kw = {}
        if noncontig:
            kw["allow_slow_non_contiguous"] = True
        Q.h.dma_start(out=out_ap, in_=in_ap, **kw).then_inc(sem, 16)
        tag = (sem, 16 * semtile.cnt)
        k = id(sem)
        all_dma[k] = tag
        src.rd[k] = tag
        if multi:
            dst.lw[k] = tag
        else:
            dst.lw = {k: tag}
            dst.rd = {}

    tiles = {}

    def sb(name, shape, dt=F32):
        h = es.enter_context(nc.sbuf_tensor("sb_" + name, list(shape), dt))
        t = T(name)
        tiles[name] = (h, t)
        return h, t

    ps_h = es.enter_context(nc.psum_tensor("ps", [128, 8, 512], F32))
    PSB = [T("psb%d" % i) for i in range(8)]

    xT32, t_x32 = sb("xT32", [128, KC, TWP])
    xTb, t_xb = sb("xTb", [128, KC, TWP], BF16)
    cbuf, t_cbuf = sb("cbuf", [128, 4, TWP])
    sqb, t_sqb = sb("sqb", [128, TWP])
    cqn, t_cqn = sb("cqn", [128, 4, TWP], BF16)
    latb, t_latb = sb("latb", [128, 4, TWP], BF16)
    bc1, t_bc1 = sb("bc1", [128, TWP])
    bc2, t_bc2 = sb("bc2", [128, TWP])
    kr32, t_kr32 = sb("kr32", [64, TWP])
    krr, t_krr = sb("krr", [64, TWP])
    krb, t_krb = sb("krb", [64, TWP], BF16)
    ropet, t_ropet = sb("ropet", [64, 2, TWP])
    ubuf, t_ubuf = sb("ubuf", [128, TWP + 3])
    uc32, t_uc32 = sb("uc32", [128, TWP])
    ucb, t_ucb = sb("ucb", [128, TWP], BF16)
    lr, t_lr = sb("lr", [128, TWP])
    li, t_li = sb("li", [128, TWP])
    la, t_la = sb("la", [128, TWP])
    lq, t_lq = sb("lq", [128, TWP])
    lh, t_lh = sb("lh", [128, TWP])
    gx, t_gx = sb("gx", [128, TWP])
    gt, t_gt = sb("gt", [128, TWP])
    sga, t_sga = sb("sga", [128, TWP])
    NKR = 2
    knr = [sb("knr%d" % i, [128, 512], BF16) for i in range(NKR)]
    vr = [sb("vr%d" % i, [128, 4, 128], BF16) for i in range(NKR)]
    krr_ring = [sb("krk%d" % i, [64, 512], BF16) for i in range(NKR)]
    ptr = [sb("pt%d" % i, [128, TWP], BF16) for i in range(NKR)]
    qn, t_qn = sb("qn", [128, TWP], BF16)
    qr32, t_qr32 = sb("qr32", [64, TWP])
    qrr, t_qrr = sb("qrr", [64, TWP])
    qrb, t_qrb = sb("qrb", [64, TWP], BF16)
    knew, t_knew = sb("knew", [128, TWP], BF16)
    vnew, t_vnew = sb("vnew", [128, 4, 128], BF16)
    wbig = [sb("wbig%d" % i, [128, KC, 256], BF16) for i in range(2)]
    w2r = [sb("w2r%d" % i, [128, 4, 512], BF16) for i in range(2)]
    wuq = [sb("wuq%d" % i, [128, 4, 192], BF16) for i in range(2)]
    wuk = [sb("wuk%d" % i, [128, 4, 128], BF16) for i in range(2)]
    wuv = [sb("wuv%d" % i, [128, 4, 128], BF16) for i in range(2)]
    wga = [sb("wga%d" % i, [128, 128], BF16) for i in range(2)]
    wgx = [sb("wgx%d" % i, [128, 128], BF16) for i in range(2)]
    NGH = 32
    gT, t_gT = sb("gT", [128, NGH, TWP], BF16)
    mrg, t_mrg = gT[:, 16:32, :], t_gT
    rec, t_rec, att, t_att = lr, t_lr, li, t_li
    sl, t_sl = sb("sl", [128, TWP])
    comb, t_comb = sb("comb", [128, TWP])
    wr_sb, t_wr = sb("wr_sb", [128, KC, NE])
    lg, t_lg = sb("lg", [128, 4, NE])
    lgt, t_lgt = sb("lgt", [128, 4, NE])
    lgm, t_lgm = sb("lgm", [128, 4, 4])
    combT, t_combT = sb("combT", [NE, TWP])
    sel, t_sel = sb("sel", [NE, NE, 128])
    stl, t_stl = sb("stl", [128, KVL])
    stk, t_stk = sb("stk", [128, RD])
    xin, t_xin = sb("xin", [128, D])
    stg, t_stg = xin, t_xin
    ident, t_ident = sb("ident", [128, 128])
    ones32, t_ones32 = sb("ones32", [128, 128])
    onesb, t_onesb = sb("onesb", [128, 128], BF16)
    rm, t_rm = sb("rm", [64, 64])
    vecs, t_vecs = sb("vecs", [128, 2 * NV])
    cA, t_cA = sb("cA", [128, 2 * KC])
    cA2, t_cA2 = sb("cA2", [128, 2 * KC])
    tmask, t_tmask = sb("tmask", [128, NT + 1])
    kbias, t_kbias = sb("kbias", [128, 4 * NT + 1])
    hst_p, t_hst_p = sb("hst_p", [128, KC])
    hst_s, t_hst_s = sb("hst_s", [128, KC])
    tail_p, t_tail_p = sb("tail_p", [128, KC, 3])
    tail_s, t_tail_s = sb("tail_s", [128, KC, 3])

    def bank(i):
        return ps_h[:, i, :], PSB[i]

    def pe_transpose(out_ap, in_ap, ident_ap):
        return nc.tensor.matmul(out_ap, in_ap, ident_ap, start=True, stop=True)

    def mm(pb, out_ap, lhsT, rhs, reads, start, stop, inc=None):
        op(PE, lambda: nc.tensor.matmul(out_ap, lhsT, rhs, start=start, stop=stop),
           reads=reads, writes=[pb], inc=(stop if inc is None else (inc or stop)), multi=not start)

    def act(out_ap, in_ap, func, reads, writes, bias=None, scale=None):
        kw = {}
        if bias is not None:
            kw["bias"] = bias
        if scale is not None:
            kw["scale"] = scale
        op(ACT, lambda: nc.scalar.activation(out=out_ap, in_=in_ap, func=func, **kw), reads, writes)

    def tt(out_ap, a, b, alu, reads, writes, E=None):
        E = E or DVE
        op(E, lambda: E.h.tensor_tensor(out=out_ap, in0=a, in1=b, op=alu), reads, writes)

    def ts(out_ap, a, s1, s2, op0, op1, reads, writes, E=None):
        E = E or DVE
        if op1 is None:
            op(E, lambda: E.h.tensor_scalar(out=out_ap, in0=a, scalar1=s1, scalar2=None, op0=op0), reads, writes)
        else:
            op(E, lambda: E.h.tensor_scalar(out=out_ap, in0=a, scalar1=s1, scalar2=s2, op0=op0, op1=op1),
               reads, writes)

    def stt(out_ap, a, s, b, op0, op1, reads, writes, E=None):
        E = E or DVE
        op(E, lambda: E.h.scalar_tensor_tensor(out=out_ap, in0=a, scalar=s, in1=b, op0=op0, op1=op1),
           reads, writes)

    def cp(out_ap, in_ap, reads, writes, E=None):
        E = E or DVE
        op(E, lambda: E.h.tensor_copy(out=out_ap, in_=in_ap), reads, writes)

    t_w = {n: T("w_" + n) for n in ("in", "uq", "uk", "uv", "ga", "gx", "o", "13", "2", "m13", "m2")}
    t_ext = T("ext")
    t_out = T("outs")
    t_x1 = [T("x1_%d" % i) for i in range(NT)]
    t_xs1 = T("xs1")
    t_kv = [T("kv_%d" % i) for i in range(NT)]
    t_kvs = [T("kvs_%d" % i) for i in range(PAST // 512 + 1)]

    def load_const(dst_h, dst_t, src_ap, shape_ap=None):
        dma(SP, shape_ap if shape_ap is not None else dst_h[:], src_ap, t_ext, dst_t, dst_t)

    load_const(ident, t_ident, ident_in[:, :])
    load_const(rm, t_rm, rm_in[:, :])
    load_const(vecs, t_vecs, vecs_in[:, :])
    load_const(tmask, t_tmask, tmask_in[:, :], tmask[:, 0:NT])
    load_const(kbias, t_kbias, kbias_in[:, :], kbias[:, 0:4 * NT])
    load_const(wr_sb, t_wr, wr_f.rearrange("(k p) e -> p k e", p=128))
    op(POOL, lambda: nc.gpsimd.memset(ones32[:], 1.0), [], [t_ones32])
    op(POOL, lambda: nc.gpsimd.memset(onesb[:], 1.0), [], [t_onesb])
    op(POOL, lambda: nc.gpsimd.memset(tmask[:, NT:NT + 1], 1.0), [], [t_tmask], multi=True)
    op(POOL, lambda: nc.gpsimd.memset(kbias[:, 4 * NT:4 * NT + 1], 0.0), [], [t_kbias], multi=True)
    load_const(sel, t_sel, sel_in[:, :, :])

    def cast_w(dst, src, tkey, nsplit):
        rows = src.shape[0]
        step = rows // nsplit
        for i in range(nsplit):
            dma(POOL, dst[i * step:(i + 1) * step, :], src[i * step:(i + 1) * step, :], t_ext, t_w[tkey],
                t_w[tkey], multi=True)

    for l in range(2):
        cast_w(w_in_b[l], w_in_f[l], "in", 4)
        cast_w(w_uq_b[l], w_uq_f[l], "uq", 1)
        cast_w(w_uk_b[l], w_uk_f[l], "uk", 1)
        cast_w(w_uv_b[l], w_uv_f[l], "uv", 1)
        cast_w(w_ga_b[l], w_ga_f[l], "ga", 1)
        cast_w(w_gx_b[l], w_gx_f[l], "gx", 1)
        cast_w(w_o_b[l], w_o_f[l], "o", 1)
    cast_w(w13_b, w13_f, "13", 4)
    cast_w(w2_b, w2_f, "2", 2)
    for e in range(NE):
        cast_w(m13_b[e], m13_f[e], "m13", 4)
        cast_w(m2_b[e], m2_f[e], "m2", 4)

    for l in range(2):
        lam = vecs[:, l * NV + VO["lam"]:l * NV + VO["lam"] + KC]
        act(cA[:, l * KC:(l + 1) * KC], lam, AF.Exp, [t_vecs], [t_cA], scale=-1.0)
        act(cA[:, l * KC:(l + 1) * KC], cA[:, l * KC:(l + 1) * KC], AF.Ln, [t_cA], [t_cA], bias=1.0)
        ts(cA2[:, l * KC:(l + 1) * KC], cA[:, l * KC:(l + 1) * KC], -16.0, None, ALU.mult, None, [t_cA], [t_cA2])
        ts(cA[:, l * KC:(l + 1) * KC], cA[:, l * KC:(l + 1) * KC], -8.0, None, ALU.mult, None, [t_cA], [t_cA])

    def vcol(l, name, j):
        c = l * NV + VO[name] + j
        return vecs[:, c:c + 1]

    rot = {"a": [0, [0, 1]], "s": [0, [2, 3]], "m": [0, [6, 7]]}

    def nbank(kind):
        r = rot[kind]
        b = r[1][r[0] % len(r[1])]
        r[0] += 1
        return b

    wb_i = [0]

    def next_wbig():
        i = wb_i[0] % 2
        wb_i[0] += 1
        return wbig[i]

    ring_i = {}

    def ring(lst, key):
        i = ring_i.get(key, 0)
        ring_i[key] = i + 1
        return lst[i % len(lst)]

    class Seq:
        pass

    P_ = Seq()
    P_.name = "p"
    P_.TW = TWP
    P_.hst, P_.t_hst, P_.tail, P_.t_tail = hst_p, t_hst_p, tail_p, t_tail_p
    P_.Kn, P_.Kr, P_.V, P_.t_kv = Kn_d, Kr_d, V_d, t_kv
    P_.lat_o, P_.kr_o = lat_o, kr_o
    S_ = Seq()
    S_.name = "s"
    S_.TW = DS
    S_.hst, S_.t_hst, S_.tail, S_.t_tail = hst_s, t_hst_s, tail_s, t_tail_s
    S_.Kn, S_.Kr, S_.V, S_.t_kv = Kns_d, Krs_d, Vs_d, t_kvs
    S_.lat_o, S_.kr_o = slat_o, skr_o

    def proj_chunk(pb_idx, W, wreads, col0, ncols, TW, rhs_h, rhs_t, nk, out_rows=None):
        pb_ap, pb = bank(pb_idx)
        for k in range(nk):
            mm(pb, pb_ap[0:ncols, 0:TW], W[:, k, col0:col0 + ncols], rhs_h[:, k, 0:TW],
               wreads + [rhs_t], k == 0, k == nk - 1)
        return pb_ap, pb

    def rmsnorm_chunks(l, gname, TW, out_b_h, out_b_t, keep32):
        pbi = nbank("m")
        pb_ap, pb = bank(pbi)
        for k in range(4):
            if _os.environ.get("KSQ", "0") == "1":
                tt(sqb[:, 0:TW], cbuf[:, k, 0:TW], cbuf[:, k, 0:TW], ALU.mult, [t_cbuf], [t_sqb])
            else:
                act(sqb[:, 0:TW], cbuf[:, k, 0:TW], AF.Square, [t_cbuf], [t_sqb])
            mm(pb, pb_ap[:, 0:TW], ones32[:, :], sqb[:, 0:TW], [t_ones32, t_sqb], k == 0, k == 3, inc=True)
        ck2(20)
        act(bc1[:, 0:TW], pb_ap[:, 0:TW], AF.Sqrt, [pb], [t_bc1], bias=1e-6, scale=1.0 / 512)
        ck2(21)
        op(DVE, lambda: nc.vector.reciprocal(out=bc1[:, 0:TW], in_=bc1[:, 0:TW]), [t_bc1], [t_bc1])
        ck2(22)
        for k in range(4):
            g = vcol(l, gname, k)
            if keep32:
                stt(cbuf[:, k, 0:TW], cbuf[:, k, 0:TW], g, bc1[:, 0:TW], ALU.mult, ALU.mult,
                    [t_cbuf, t_bc1, t_vecs], [t_cbuf])
                act(out_b_h[:, k, 0:TW], cbuf[:, k, 0:TW], AF.Copy, [t_cbuf], [out_b_t])
            else:
                stt(out_b_h[:, k, 0:TW], cbuf[:, k, 0:TW], g, bc1[:, 0:TW], ALU.mult, ALU.mult,
                    [t_cbuf, t_bc1, t_vecs], [out_b_t])

    def rope_apply(src32_h, src32_t, tmp_h, tmp_t, TW, out_b_h, out_b_t, keep32):
        pbi = nbank("m")
        pb_ap, pb = bank(pbi)
        mm(pb, pb_ap[0:64, 0:TW], rm[:, :], src32_h[:, 0:TW], [t_rm, src32_t], True, True)
        tt(tmp_h[:, 0:TW], pb_ap[0:64, 0:TW], ropet[:, 1, 0:TW], ALU.mult, [pb, t_ropet], [tmp_t])
        tt(src32_h[:, 0:TW], src32_h[:, 0:TW], ropet[:, 0, 0:TW], ALU.mult, [src32_t, t_ropet], [src32_t])
        tt(src32_h[:, 0:TW], src32_h[:, 0:TW], tmp_h[:, 0:TW], ALU.add, [src32_t, tmp_t], [src32_t])
        act(out_b_h[:, 0:TW], src32_h[:, 0:TW], AF.Copy, [src32_t], [out_b_t])

    def out_transposed(src_h, src_t, nfeat_chunks, fw, TW, dst_ap_fn, stage_h, stage_t):
        ntb = (TW + 127) // 128
        for tb in range(ntb):
            ntok = min(128, TW - tb * 128)
            for c0 in range(0, nfeat_chunks, 4):
                pbi = nbank("m")
                pb_ap, pb = bank(pbi)
                ncs = min(4, nfeat_chunks - c0)
                for c in range(ncs):
                    if len(src_h.shape) == 3:
                        s_ap = src_h[0:fw, c0 + c, tb * 128:tb * 128 + ntok]
                    else:
                        s_ap = src_h[0:fw, tb * 128:tb * 128 + ntok]
                    op(PE, lambda s_ap=s_ap, c=c: pe_transpose(
                        pb_ap[0:ntok, c * fw:(c + 1) * fw], s_ap, ident[0:fw, 0:fw]),
                        reads=[src_t, t_ident], writes=[pb], inc=True, multi=(c > 0))
                act(stage_h[0:ntok, c0 * fw:(c0 + ncs) * fw], pb_ap[0:ntok, 0:ncs * fw], AF.Copy, [pb], [stage_t])
            dma(POOL, dst_ap_fn(tb, ntok), stage_h[0:ntok, 0:nfeat_chunks * fw], stage_t, t_out, stage_t, multi=True)

    def layer_norm(l, gname, bname, TW, mask_col=None):
        pb1_ap, pb1 = bank(nbank("m"))
        pb2_ap, pb2 = bank(nbank("m"))
        for k in range(KC):
            mm(pb1, pb1_ap[:, 0:TW], ones32[:, :], xT32[:, k, 0:TW], [t_ones32, t_x32], k == 0, k == KC - 1)
        for k in range(KC):
            act(sqb[:, 0:TW], xT32[:, k, 0:TW], AF.Square, [t_x32], [t_sqb])
            mm(pb2, pb2_ap[:, 0:TW], ones32[:, :], sqb[:, 0:TW], [t_ones32, t_sqb], k == 0, k == KC - 1, inc=True)
        ts(bc1[:, 0:TW], pb1_ap[:, 0:TW], 1.0 / D, None, ALU.mult, None, [pb1], [t_bc1])
        tt(bc2[:, 0:TW], bc1[:, 0:TW], bc1[:, 0:TW], ALU.mult, [t_bc1], [t_bc2])
        stt(bc2[:, 0:TW], pb2_ap[:, 0:TW], 1.0 / D, bc2[:, 0:TW], ALU.mult, ALU.subtract, [pb2, t_bc2], [t_bc2])
        act(bc2[:, 0:TW], bc2[:, 0:TW], AF.Sqrt, [t_bc2], [t_bc2], bias=1e-5, scale=1.0)
        op(DVE, lambda: nc.vector.reciprocal(out=bc2[:, 0:TW], in_=bc2[:, 0:TW]), [t_bc2], [t_bc2])
        stt(bc1[:, 0:TW], bc1[:, 0:TW], -1.0, bc2[:, 0:TW], ALU.mult, ALU.mult, [t_bc1, t_bc2], [t_bc1])
        for k in range(KC):
            tt(xT32[:, k, 0:TW], xT32[:, k, 0:TW], bc2[:, 0:TW], ALU.mult, [t_x32, t_bc2], [t_x32])
            tt(xT32[:, k, 0:TW], xT32[:, k, 0:TW], bc1[:, 0:TW], ALU.add, [t_x32, t_bc1], [t_x32])
            ts(xT32[:, k, 0:TW], xT32[:, k, 0:TW], vcol(l, gname, k), vcol(l, bname, k), ALU.mult, ALU.add,
               [t_x32, t_vecs], [t_x32])
            if mask_col is not None:
                ts(xT32[:, k, 0:TW], xT32[:, k, 0:TW], mask_col, None, ALU.mult, None, [t_x32, t_tmask], [t_x32])
            act(xTb[:, k, 0:TW], xT32[:, k, 0:TW], AF.Copy, [t_x32], [t_xb])

    def load_w(dst, src_ap, tkey, shape_ap=None):
        h, t = dst
        dma(SP, shape_ap if shape_ap is not None else h[:], src_ap, t_w[tkey], t, t)
        return h, t

    def win_cols(l, c0, n):
        return w_in_b[l].rearrange("(k p) c -> p k c", p=128)[:, :, c0:c0 + n]

    def mix_tile(l, sq, t, mode):
        TW = sq.TW
        full = mode == "full"
        pos0 = t * TW if sq is P_ else PAST
        tcol = tmask[:, t:t + 1] if sq is P_ else tmask[:, NT:NT + 1]
        if sq is P_:
            dma(SP, ropet[:, :, 0:TW], rope_in[:, :, pos0:pos0 + TW], t_ext, t_ropet, t_ropet)
        else:
            dma(SP, ropet[:, :, 0:TW], ropes_in[:, :, :], t_ext, t_ropet, t_ropet)
        ck2(10)
        if full:
            for hf in range(2):
                w_a = load_w(next_wbig(), win_cols(l, hf * 256, 256), "in")
                for k2 in range(2):
                    k = hf * 2 + k2
                    pb_ap, pb = proj_chunk(nbank("a"), w_a[0], [w_a[1]], k2 * 128, 128, TW, xTb, t_xb, KC)
                    act(cbuf[:, k, 0:TW], pb_ap[:, 0:TW], AF.Copy, [pb], [t_cbuf])
            ck2(11)
            rmsnorm_chunks(l, "qg", TW, cqn, t_cqn, False)
            ck2(23)
        for hf in range(2):
            w_b = load_w(next_wbig(), win_cols(l, 512 + hf * 256, 256), "in")
            for k2 in range(2):
                k = hf * 2 + k2
                pb_ap, pb = proj_chunk(nbank("a"), w_b[0], [w_b[1]], k2 * 128, 128, TW, xTb, t_xb, KC)
                act(cbuf[:, k, 0:TW], pb_ap[:, 0:TW], AF.Copy, [pb], [t_cbuf])
        wc_buf = next_wbig()
        w_c = load_w(wc_buf, win_cols(l, 1024, 128), "in", wc_buf[0][:, :, 0:128])
        rmsnorm_chunks(l, "kvg", TW, latb, t_latb, True)
        ck(2)
        out_transposed(cbuf, t_cbuf, 4, 128, TW,
                       lambda tb, ntok: sq.lat_o[l, (pos0 - (PAST if sq is S_ else 0)) + tb * 128:
                                                  (pos0 - (PAST if sq is S_ else 0)) + tb * 128 + ntok, :],
                       stl, t_stl)
        pb_ap, pb = proj_chunk(nbank("a"), w_c[0], [w_c[1]], 0, 64, TW, xTb, t_xb, KC)
        act(kr32[:, 0:TW], pb_ap[0:64, 0:TW], AF.Copy, [pb], [t_kr32])
        rope_apply(kr32, t_kr32, krr, t_krr, TW, krb, t_krb, True)
        out_transposed(kr32, t_kr32, 1, 64, TW,
                       lambda tb, ntok: sq.kr_o[l, (pos0 - (PAST if sq is S_ else 0)) + tb * 128:
                                                 (pos0 - (PAST if sq is S_ else 0)) + tb * 128 + ntok, :],
                       stk, t_stk)
        kvt = sq.t_kv[t if sq is P_ else PAST // 512]
        dma(POOL, sq.Kr[:, pos0:pos0 + TW], krb[:, 0:TW], t_krb, kvt, t_krb, multi=True)

        ck(3)
        for j in range(KC):
            wj = load_w(next_wbig(), win_cols(l, 1152 + j * 512, 256), "in")
            wgaj = load_w(ring(wga, "wga"), w_ga_b[l, j * 128:(j + 1) * 128, :], "ga")
            wgxj = load_w(ring(wgx, "wgx"), w_gx_b[l, j * 128:(j + 1) * 128, :], "gx")
            pb_ap, pb = proj_chunk(nbank("a"), wj[0], [wj[1]], 0, 128, TW, xTb, t_xb, KC)
            cp(ubuf[:, 0:3], sq.tail[:, j, :], [sq.t_tail], [t_ubuf])
            act(ubuf[:, 3:3 + TW], pb_ap[:, 0:TW], AF.Copy, [pb], [t_ubuf])
            cp(sq.tail[:, j, :], ubuf[:, TW:TW + 3], [t_ubuf], [sq.t_tail])
            ts(uc32[:, 0:TW], ubuf[:, 0:TW], vcol(l, "cw", 0 * KC + j), vcol(l, "cb", j), ALU.mult, ALU.add,
               [t_ubuf, t_vecs], [t_uc32])
            for tap in range(1, 4):
                stt(uc32[:, 0:TW], ubuf[:, tap:tap + TW], vcol(l, "cw", tap * KC + j), uc32[:, 0:TW],
                    ALU.mult, ALU.add, [t_ubuf, t_uc32, t_vecs], [t_uc32])
            act(ucb[:, 0:TW], uc32[:, 0:TW], AF.Copy, [t_uc32], [t_ucb])
            pbr_ap, pbr = bank(nbank("a"))
            mm(pbr, pbr_ap[:, 0:TW], wgaj[0][:, :], ucb[:, 0:TW], [wgaj[1], t_ucb], True, True)
            act(lr[:, 0:TW], pbr_ap[:, 0:TW], AF.Sigmoid, [pbr, t_vecs], [t_lr], bias=vcol(l, "ba", j))
            pbi_ap, pbi = bank(nbank("a"))
            mm(pbi, pbi_ap[:, 0:TW], wgxj[0][:, :], ucb[:, 0:TW], [wgxj[1], t_ucb], True, True)
            act(li[:, 0:TW], pbi_ap[:, 0:TW], AF.Sigmoid, [pbi, t_vecs], [t_li], bias=vcol(l, "bx", j))
            act(la[:, 0:TW], lr[:, 0:TW], AF.Exp, [t_lr, t_cA], [t_la], scale=cA[:, l * KC + j:l * KC + j + 1])
            act(lq[:, 0:TW], lr[:, 0:TW], AF.Exp, [t_lr, t_cA2], [t_lq], scale=cA2[:, l * KC + j:l * KC + j + 1])
            act(lq[:, 0:TW], lq[:, 0:TW], AF.Sqrt, [t_lq], [t_lq], bias=1.0, scale=-1.0)
            tt(li[:, 0:TW], li[:, 0:TW], uc32[:, 0:TW], ALU.mult, [t_li, t_uc32], [t_li])
            stt(li[:, 0:TW], li[:, 0:TW], tcol, lq[:, 0:TW], ALU.mult, ALU.mult, [t_li, t_lq, t_tmask], [t_li])
            op(DVE, lambda j=j: nc.vector.tensor_tensor_scan(out=lh[:, 0:TW], data0=la[:, 0:TW], data1=li[:, 0:TW],
                                                             initial=sq.hst[:, j:j + 1], op0=ALU.mult, op1=ALU.add),
               [t_la, t_li, sq.t_hst], [t_lh])
            cp(sq.hst[:, j:j + 1], lh[:, TW - 1:TW], [t_lh], [sq.t_hst])
            if full:
                pb_ap, pb = proj_chunk(nbank("a"), wj[0], [wj[1]], 128, 128, TW, xTb, t_xb, KC)
                act(gx[:, 0:TW], pb_ap[:, 0:TW], AF.Copy, [pb], [t_gx])
                tt(gt[:, 0:TW], gx[:, 0:TW], gx[:, 0:TW], ALU.mult, [t_gx], [t_gt])
                ts(gt[:, 0:TW], gt[:, 0:TW], 0.044715, 1.0, ALU.mult, ALU.add, [t_gt], [t_gt])
                tt(gt[:, 0:TW], gt[:, 0:TW], gx[:, 0:TW], ALU.mult, [t_gt, t_gx], [t_gt])
                act(gt[:, 0:TW], gt[:, 0:TW], AF.Sigmoid, [t_gt], [t_gt], scale=1.5957691216057308)
                tt(gt[:, 0:TW], gt[:, 0:TW], gx[:, 0:TW], ALU.mult, [t_gt, t_gx], [t_gt])
                wj2 = load_w(next_wbig(), win_cols(l, 1152 + j * 512 + 256, 256), "in")
                pb_ap, pb = proj_chunk(nbank("a"), wj2[0], [wj2[1]], 0, 128, TW, xTb, t_xb, KC)
                act(sga[:, 0:TW], pb_ap[:, 0:TW], AF.Sigmoid, [pb], [t_sga])
                tt(gt[:, 0:TW], gt[:, 0:TW], lh[:, 0:TW], ALU.mult, [t_gt, t_lh], [t_gt])
                tt(mrg[:, j, 0:TW], gt[:, 0:TW], sga[:, 0:TW], ALU.mult, [t_gt, t_sga], [t_mrg])
                pb_ap, pb = proj_chunk(nbank("a"), wj2[0], [wj2[1]], 128, 128, TW, xTb, t_xb, KC)
                act(sq.sgb_all[:, j, 0:TW], pb_ap[:, 0:TW], AF.Sigmoid, [pb], [sq.t_sgb_all])

        ck(4)
        ntb = (TW + 127) // 128
        if sq is P_:
            prev_tiles = list(range(t))
        else:
            prev_tiles = list(range(PAST // 512))
        for j in range(H):
            wukj = load_w(ring(wuk, "wuk"), w_uk_b[l].rearrange("(k p) c -> p k c", p=128)[:, :, j * 128:(j + 1) * 128], "uk")
            wuvj = load_w(ring(wuv, "wuv"), w_uv_b[l].rearrange("(k p) c -> p k c", p=128)[:, :, j * 128:(j + 1) * 128], "uv")
            pb_ap, pb = proj_chunk(nbank("a"), wukj[0], [wukj[1]], 0, 128, TW, latb, t_latb, 4)
            act(knew[:, 0:TW], pb_ap[:, 0:TW], AF.Copy, [pb], [t_knew])
            dma(POOL, sq.Kn[j, :, pos0:pos0 + TW], knew[:, 0:TW], t_knew, kvt, t_knew, multi=True)
            for tb in range(ntb):
                ntok = min(128, TW - tb * 128)
                pb_ap, pb = bank(nbank("a"))
                for k in range(4):
                    mm(pb, pb_ap[0:ntok, 0:128], latb[:, k, tb * 128:tb * 128 + ntok], wuvj[0][:, k, :],
                       [t_latb, wuvj[1]], k == 0, k == 3)
                act(vnew[0:ntok, tb, :], pb_ap[0:ntok, 0:128], AF.Copy, [pb], [t_vnew])
            if TW >= 128:
                dma(POOL, sq.V[j, pos0:pos0 + TW, :].rearrange("(b p) f -> p b f", p=128), vnew[:, 0:ntb, :],
                    t_vnew, kvt, t_vnew, multi=True)
            else:
                dma(POOL, sq.V[j, pos0:pos0 + TW, :], vnew[0:TW, 0, :], t_vnew, kvt, t_vnew, multi=True)
            if not full:
                continue
            wuqj = load_w(ring(wuq, "wuq"), w_uq_b[l].rearrange("(k p) c -> p k c", p=128)[:, :, j * 192:(j + 1) * 192], "uq")
            pb_ap, pb = proj_chunk(nbank("a"), wuqj[0], [wuqj[1]], 0, 128, TW, cqn, t_cqn, 4)
            act(qn[:, 0:TW], pb_ap[:, 0:TW], AF.Copy, [pb], [t_qn])
            pb_ap, pb = proj_chunk(nbank("a"), wuqj[0], [wuqj[1]], 128, 64, TW, cqn, t_cqn, 4)
            act(qr32[:, 0:TW], pb_ap[0:64, 0:TW], AF.Copy, [pb], [t_qr32])
            rope_apply(qr32, t_qr32, qrr, t_qrr, TW, qrb, t_qrb, False)
            blocks = []
            if TW == 512:
                for d in range(4):
                    blocks.append(("new", d, 128, 128 * d, True, kbias[:, 4 * t + d:4 * t + d + 1]))
            else:
                blocks.append(("new", 0, TW, 0, False, kbias[:, 4 * NT:4 * NT + 1]))
            for pt in prev_tiles:
                for d in range(4):
                    bcol = kbias[:, 4 * pt + d:4 * pt + d + 1] if sq is P_ else kbias[:, 4 * NT:4 * NT + 1]
                    blocks.append(("old", (pt, d), 128, 0, False, bcol))
            po_ap, po = bank(4)
            pd_ap, pd = bank(5)
            cur = {}
            for bi, (kind, ix, nk, c_lo, corner, bcol) in enumerate(blocks):
                if kind == "new":
                    kn_ap, kn_t = knew[:, ix * 128:ix * 128 + nk], t_knew
                    kr_ap, kr_t = krb[:, ix * 128:ix * 128 + nk], t_krb
                    v_ap, v_t = vnew[0:nk, ix, :], t_vnew
                else:
                    pt, d = ix
                    if d == 0:
                        kh, kt = ring(knr, "knr")
                        vh, vt = ring(vr, "vr")
                        kvt_o = sq.t_kv[pt]
                        dma(SP, kh[:, :], sq.Kn[j, :, pt * 512:(pt + 1) * 512], kvt_o, kt, kt)
                        dma(SP, vh[:, :, :], sq.V[j, pt * 512:(pt + 1) * 512, :].rearrange("(b p) f -> p b f", p=128),
                            kvt_o, vt, vt)
                        rh, rt = ring(krr_ring, "krk")
                        dma(SP, rh[:, :], sq.Kr[:, pt * 512:(pt + 1) * 512], kvt_o, rt, rt)
                        cur["k"], cur["v"], cur["r"] = (kh, kt), (vh, vt), (rh, rt)
                    kn_ap, kn_t = cur["k"][0][:, d * 128:(d + 1) * 128], cur["k"][1]
                    kr_ap, kr_t = cur["r"][0][:, d * 128:(d + 1) * 128], cur["r"][1]
                    v_ap, v_t = cur["v"][0][:, d, :], cur["v"][1]
                ps_ap, psb = bank(nbank("s"))
                mm(psb, ps_ap[0:nk, c_lo:TW], kn_ap, qn[:, c_lo:TW], [kn_t, t_qn], True, False)
                mm(psb, ps_ap[0:nk, c_lo:TW], kr_ap, qrb[:, c_lo:TW], [kr_t, t_qrb], False, True)
                ph, ptt = ring(ptr, "pt")
                act(ph[0:nk, c_lo:TW], ps_ap[0:nk, c_lo:TW], AF.Exp, [psb, t_kbias], [ptt], bias=bcol[0:nk, :],
                    scale=SCALE)
                if corner:
                    op(POOL, lambda ph=ph, c_lo=c_lo: nc.gpsimd.memset(ph[64:128, c_lo:c_lo + 64], 0.0), [], [ptt],
                       multi=True)
                last = bi == len(blocks) - 1
                mm(po, po_ap[:, c_lo:TW], v_ap, ph[0:nk, c_lo:TW], [v_t, ptt], bi == 0, last)
                mm(pd, pd_ap[:, c_lo:TW], onesb[0:nk, :], ph[0:nk, c_lo:TW], [t_onesb, ptt], bi == 0, last, inc=True)
            op(DVE, lambda: nc.vector.reciprocal(out=rec[:, 0:TW], in_=pd_ap[:, 0:TW]), [pd], [t_rec])
            tt(att[:, 0:TW], po_ap[:, 0:TW], rec[:, 0:TW], ALU.mult, [po, t_rec], [t_att])
            tt(att[:, 0:TW], att[:, 0:TW], sq.sgb_all[:, j, 0:TW], ALU.mult, [t_att, sq.t_sgb_all], [t_att])
            tt(mrg[:, j, 0:TW], mrg[:, j, 0:TW], att[:, 0:TW], ALU.add, [t_mrg, t_att], [t_mrg])
        if not full:
            return
        ck(5)
        wog = w_o_b[l].rearrange("(k p) c -> p k c", p=128)
        for g in range(KC // 2):
            wo = load_w(next_wbig(), wog[:, :, g * 256:(g + 1) * 256], "o")
            for m in range(2):
                pb_ap, pb = proj_chunk(nbank("a"), wo[0], [wo[1]], m * 128, 128, TW, mrg, t_mrg, KC)
                k = g * 2 + m
                stt(xT32[:, k, 0:TW], xT32[:, k, 0:TW], ALPHA, pb_ap[:, 0:TW], ALU.mult, ALU.add, [t_x32, pb], [t_x32])

    sgb_all, t_sgb_all = gT[:, 0:16, :], t_gT
    P_.sgb_all, P_.t_sgb_all = sgb_all, t_sgb_all
    S_.sgb_all, S_.t_sgb_all = sgb_all, t_sgb_all
    P_.wj_cache, S_.wj_cache = {}, {}

    def ffn_expert(W13, W2, t13key, t2key, nfc, TW, comb_ap, comb_t, first):
        halves = [(0, nfc)] if nfc <= NGH else [(0, NGH), (NGH, nfc)]
        w13v = W13.rearrange("(k p) c -> p k c", p=128)
        for (c0, c1) in halves:
            n = c1 - c0
            for m in range(c0, c1):
                wt = load_w(next_wbig(), w13v[:, :, m * 256:(m + 1) * 256], t13key)
                p1_ap, p1 = proj_chunk(nbank("a"), wt[0], [wt[1]], 0, 128, TW, xTb, t_xb, KC)
                p3_ap, p3 = proj_chunk(nbank("s"), wt[0], [wt[1]], 128, 128, TW, xTb, t_xb, KC)
                act(sl[:, 0:TW], p1_ap[:, 0:TW], AF.Silu, [p1], [t_sl])
                tt(gT[:, m - c0, 0:TW], sl[:, 0:TW], p3_ap[:, 0:TW], ALU.mult, [t_sl, p3], [t_gT])
            for g in range(KC // 4):
                pbs = [bank(b) for b in (4, 5, 6, 7)]
                for kk in range(0, n, 4):
                    nk4 = min(4, n - kk)
                    w2t = ring(w2r, "w2r")
                    r0 = (c0 + kk) * 128
                    load_w(w2t, W2[r0:r0 + nk4 * 128, g * 512:(g + 1) * 512].rearrange("(k p) c -> p k c", p=128),
                           t2key, w2t[0][:, 0:nk4, :])
                    for k4 in range(nk4):
                        for m in range(4):
                            mm(pbs[m][1], pbs[m][0][:, 0:TW], w2t[0][:, k4, m * 128:(m + 1) * 128],
                               gT[:, kk + k4, 0:TW], [w2t[1], t_gT], kk + k4 == 0, kk + k4 == n - 1,
                               inc=(k4 == nk4 - 1 and m == 3))
                for m in range(4):
                    k = g * 4 + m
                    if comb_ap is None:
                        tt(xT32[:, k, 0:TW], xT32[:, k, 0:TW], pbs[m][0][:, 0:TW], ALU.add, [t_x32, pbs[m][1]], [t_x32])
                    else:
                        tt(sl[:, 0:TW], pbs[m][0][:, 0:TW], comb_ap, ALU.mult, [pbs[m][1], comb_t], [t_sl])
                        tt(xT32[:, k, 0:TW], xT32[:, k, 0:TW], sl[:, 0:TW], ALU.add, [t_x32, t_sl], [t_x32])

    def ffn_tile(l, TW):
        if l == 0:
            for k in range(KC):
                ts(xT32[:, k, 0:TW], xT32[:, k, 0:TW], ALPHA, None, ALU.mult, None, [t_x32], [t_x32])
            ffn_expert(w13_b, w2_b, "13", "2", NFC, TW, None, None, True)
            return
        ntb = (TW + 127) // 128
        for tb in range(ntb):
            ntok = min(128, TW - tb * 128)
            pb_ap, pb = bank(nbank("m"))
            for k in range(KC):
                mm(pb, pb_ap[0:ntok, 0:NE], xT32[:, k, tb * 128:tb * 128 + ntok], wr_sb[:, k, :], [t_x32, t_wr],
                   k == 0, k == KC - 1)
            act(lg[0:ntok, tb, :], pb_ap[0:ntok, 0:NE], AF.Copy, [pb], [t_lg])
        npart = min(128, TW)
        L = lg[0:npart, 0:ntb, :]
        m1 = lgm[0:npart, 0:ntb, 0:1]
        m2 = lgm[0:npart, 0:ntb, 1:2]
        g1 = lgm[0:npart, 0:ntb, 2:3]
        g2 = lgm[0:npart, 0:ntb, 3:4]
        for tb in range(ntb):
            op(DVE, lambda tb=tb: nc.vector.reduce_max(out=lgm[0:npart, tb, 0:1], in_=lg[0:npart, tb, :],
                                                       axis=mybir.AxisListType.X), [t_lg], [t_lgm], multi=True)
            ts(lgt[0:npart, tb, :], lg[0:npart, tb, :], lgm[0:npart, tb, 0:1], None, ALU.is_equal, None,
               [t_lg, t_lgm], [t_lgt], )
            stt(sl[0:npart, tb * NE:(tb + 1) * NE], lgt[0:npart, tb, :], -1e30, lg[0:npart, tb, :], ALU.mult, ALU.add,
                [t_lgt, t_lg], [t_sl])
            op(DVE, lambda tb=tb: nc.vector.reduce_max(out=lgm[0:npart, tb, 1:2], in_=sl[0:npart, tb * NE:(tb + 1) * NE],
                                                       axis=mybir.AxisListType.X), [t_sl], [t_lgm], multi=True)
            ts(sl[0:npart, tb * NE:(tb + 1) * NE], sl[0:npart, tb * NE:(tb + 1) * NE], lgm[0:npart, tb, 1:2], None,
               ALU.is_equal, None, [t_sl, t_lgm], [t_sl])
            tt(lgm[0:npart, tb, 2:3], lgm[0:npart, tb, 0:1], lgm[0:npart, tb, 1:2], ALU.subtract, [t_lgm], [t_lgm])
            act(lgm[0:npart, tb, 2:3], lgm[0:npart, tb, 2:3], AF.Sigmoid, [t_lgm], [t_lgm])
            ts(lgm[0:npart, tb, 3:4], lgm[0:npart, tb, 2:3], -1.0, 1.0, ALU.mult, ALU.add, [t_lgm], [t_lgm])
            ts(lgt[0:npart, tb, :], lgt[0:npart, tb, :], lgm[0:npart, tb, 2:3], None, ALU.mult, None,
               [t_lgt, t_lgm], [t_lgt])
            stt(lgt[0:npart, tb, :], sl[0:npart, tb * NE:(tb + 1) * NE], lgm[0:npart, tb, 3:4], lgt[0:npart, tb, :],
                ALU.mult, ALU.add, [t_sl, t_lgm, t_lgt], [t_lgt])
            pb_ap, pb = bank(nbank("m"))
            op(PE, lambda tb=tb, pb_ap=pb_ap: pe_transpose(pb_ap[0:NE, 0:npart], lgt[0:npart, tb, :],
                                                                 ident[0:npart, 0:npart]),
               [t_lgt, t_ident], [pb])
            act(combT[:, tb * 128:tb * 128 + npart], pb_ap[0:NE, 0:npart], AF.Copy, [pb], [t_combT])
        for k in range(KC):
            ts(xT32[:, k, 0:TW], xT32[:, k, 0:TW], ALPHA, None, ALU.mult, None, [t_x32], [t_x32])
        for e in range(NE):
            pb_ap, pb = bank(nbank("a"))
            mm(pb, pb_ap[:, 0:TW], sel[:, e, :], combT[:, 0:TW], [t_sel, t_combT], True, True)
            act(comb[:, 0:TW], pb_ap[:, 0:TW], AF.Copy, [pb], [t_comb])
            ffn_expert(m13_b[e], m2_b[e], "m13", "m2", NFCE, TW, comb[:, 0:TW], t_comb, e == 0)

    def load_x_tokmajor(src_ap_fn, TW):
        import os
        ksub = 5
        ntb = (TW + 127) // 128
        for tb in range(ntb):
            ntok = min(128, TW - tb * 128)
            dma(SP, xin[0:ntok, :], src_ap_fn(tb, ntok), t_ext, t_xin, t_xin)
            if ksub < 1:
                continue
            for k0 in range(0, KC, 4):
                pb_ap, pb = bank(nbank("m"))
                for k in range(4):
                    op(PE, lambda k=k, k0=k0, pb_ap=pb_ap, ntok=ntok: pe_transpose(
                        pb_ap[:, k * 128:k * 128 + ntok], xin[0:ntok, (k0 + k) * 128:(k0 + k + 1) * 128],
                        ident[0:ntok, 0:ntok]), [t_xin, t_ident], [pb], multi=(k > 0))
                if ksub < 2:
                    continue
                for k in range(4):
                    act(xT32[:, k0 + k, tb * 128:tb * 128 + ntok], pb_ap[:, k * 128:k * 128 + ntok], AF.Copy, [pb], [t_x32])
                    if ksub < 3:
                        continue
                    if ksub == 4:
                        act(xTb[:, k0 + k, tb * 128:tb * 128 + ntok], pb_ap[:, k * 128:k * 128 + ntok], AF.Copy, [pb], [t_xb])
                    elif ksub == 5:
                        cp(xTb[:, k0 + k, tb * 128:tb * 128 + ntok], xT32[:, k0 + k, tb * 128:tb * 128 + ntok], [t_x32], [t_xb])
                    else:
                        cp(xTb[:, k0 + k, tb * 128:tb * 128 + ntok], pb_ap[:, k * 128:k * 128 + ntok], [pb], [t_xb])

    def full_layer_tile(l, sq, t):
        TW = sq.TW
        mix_tile(l, sq, t, "full")
        ck(6)
        layer_norm(l, "l1g", "l1b", TW)
        ck(7)
        ffn_tile(l, TW)
        ck(8)
        mcol = None
        if l == 0:
            mcol = tmask[:, t:t + 1] if sq is P_ else None
        layer_norm(l, "l2g", "l2b", TW, mask_col=mcol)

    def store_x1(sq, t):
        TW = sq.TW
        if sq is P_:
            dma(POOL, x1f[t], xT32[:, :, :], t_x32, t_x1[t], t_x32, multi=True)
            dma(POOL, x1b[t], xTb[:, :, :], t_xb, t_x1[t], t_xb, multi=True)
        else:
            dma(POOL, xs1f[:, :, :], xT32[:, :, 0:TW], t_x32, t_xs1, t_x32, multi=True)
            dma(POOL, xs1b[:, :, :], xTb[:, :, 0:TW], t_xb, t_xs1, t_xb, multi=True)

    def load_x1(sq, t, need32):
        TW = sq.TW
        if sq is P_:
            if need32:
                dma(SP, xT32[:, :, :], x1f[t], t_x1[t], t_x32, t_x32)
            dma(SP, xTb[:, :, :], x1b[t], t_x1[t], t_xb, t_xb)
        else:
            dma(SP, xT32[:, :, 0:TW], xs1f[:, :, :], t_xs1, t_x32, t_x32)
            dma(SP, xTb[:, :, 0:TW], xs1b[:, :, :], t_xs1, t_xb, t_xb)

    def store_state(sq, l, lo, co):
        dma(POOL, lo[l], sq.hst[:, :], sq.t_hst, t_out, sq.t_hst, multi=True)
        dma(POOL, co[l], sq.tail[:, :, :], sq.t_tail, t_out, sq.t_tail, multi=True)

    def sample_cache_kv(l):
        for pt in range(PAST // 512):
            for tb in range(4):
                r0 = pt * 512 + tb * 128
                dma(SP, xin[:, 0:KVL], clat[l, r0:r0 + 128, :], t_ext, t_xin, t_xin)
                dma(SP, xin[:, KVL:KVL + RD], ckr[l, r0:r0 + 128, :], t_ext, t_xin, t_xin)
                pb_ap, pb = bank(nbank("m"))
                for k in range(4):
                    op(PE, lambda k=k, pb_ap=pb_ap: pe_transpose(
                        pb_ap[:, k * 128:(k + 1) * 128], xin[:, k * 128:(k + 1) * 128], ident[:, :]),
                        [t_xin, t_ident], [pb], multi=(k > 0))
                for k in range(4):
                    act(latb[:, k, tb * 128:(tb + 1) * 128], pb_ap[:, k * 128:(k + 1) * 128], AF.Copy, [pb], [t_latb])
                pb_ap, pb = bank(nbank("m"))
                op(PE, lambda pb_ap=pb_ap: pe_transpose(pb_ap[0:64, 0:128], xin[:, KVL:KVL + RD], ident[:, :]),
                   [t_xin, t_ident], [pb])
                act(krb[:, tb * 128:(tb + 1) * 128], pb_ap[0:64, 0:128], AF.Copy, [pb], [t_krb])
            kvt = t_kvs[pt]
            dma(POOL, Krs_d[:, pt * 512:(pt + 1) * 512], krb[:, :], t_krb, kvt, t_krb, multi=True)
            for j in range(H):
                wukj = load_w(ring(wuk, "wuk"), w_uk_b[l].rearrange("(k p) c -> p k c", p=128)[:, :, j * 128:(j + 1) * 128], "uk")
                wuvj = load_w(ring(wuv, "wuv"), w_uv_b[l].rearrange("(k p) c -> p k c", p=128)[:, :, j * 128:(j + 1) * 128], "uv")
                pb_ap, pb = proj_chunk(nbank("a"), wukj[0], [wukj[1]], 0, 128, 512, latb, t_latb, 4)
                act(knew[:, :], pb_ap[:, :], AF.Copy, [pb], [t_knew])
                dma(POOL, Kns_d[j, :, pt * 512:(pt + 1) * 512], knew[:, :], t_knew, kvt, t_knew, multi=True)
                for tb in range(4):
                    pb_ap, pb = bank(nbank("a"))
                    for k in range(4):
                        mm(pb, pb_ap[:, 0:128], latb[:, k, tb * 128:(tb + 1) * 128], wuvj[0][:, k, :],
                           [t_latb, wuvj[1]], k == 0, k == 3)
                    act(vnew[:, tb, :], pb_ap[:, 0:128], AF.Copy, [pb], [t_vnew])
                dma(POOL, Vs_d[j, pt * 512:(pt + 1) * 512, :].rearrange("(b p) f -> p b f", p=128), vnew[:, :, :],
                    t_vnew, kvt, t_vnew, multi=True)

    def ck(n):
        if kstop is not None and n >= kstop:
            raise _Stop()

    def program():
        op(POOL, lambda: nc.gpsimd.memset(hst_p[:], 0.0), [], [t_hst_p])
        op(POOL, lambda: nc.gpsimd.memset(tail_p[:], 0.0), [], [t_tail_p])
        ck(0)
        for t in range(NT):
            load_x_tokmajor(lambda tb, ntok, t=t: x_in[t * TWP + tb * 128:t * TWP + tb * 128 + ntok, :], TWP)
            ck(1)
            full_layer_tile(0, P_, t)
            store_x1(P_, t)
            ck(10)
        store_state(P_, 0, lru_o, conv_o)
        dma(SP, hst_s[:, :], slru[0], t_ext, t_hst_s, t_hst_s)
        dma(SP, tail_s[:, :, :], sconv[0], t_ext, t_tail_s, t_tail_s)
        sample_cache_kv(0)
        load_x_tokmajor(lambda tb, ntok: xs_in[0:ntok, :], DS)
        full_layer_tile(0, S_, 0)
        store_x1(S_, 0)
        store_state(S_, 0, slru_o, sconv_o)
        op(POOL, lambda: nc.gpsimd.memset(hst_p[:], 0.0), [], [t_hst_p])
        op(POOL, lambda: nc.gpsimd.memset(tail_p[:], 0.0), [], [t_tail_p])
        for t in range(NT):
            if t < NH1:
                load_x1(P_, t, False)
                mix_tile(1, P_, t, "partial")
            else:
                load_x1(P_, t, True)
                full_layer_tile(1, P_, t)
                out_transposed(xT32, t_x32, KC, 128, TWP,
                               lambda tb, ntok, t=t: y_o[(t - NH1) * TWP + tb * 128:(t - NH1) * TWP + tb * 128 + ntok, :],
                               stg, t_stg)
        store_state(P_, 1, lru_o, conv_o)
        dma(SP, hst_s[:, :], slru[1], t_ext, t_hst_s, t_hst_s)
        dma(SP, tail_s[:, :, :], sconv[1], t_ext, t_tail_s, t_tail_s)
        sample_cache_kv(1)
        load_x1(S_, 0, True)
        full_layer_tile(1, S_, 0)
        out_transposed(xT32, t_x32, KC, 128, DS, lambda tb, ntok: ys_o[0:ntok, :], stg, t_stg)
        store_state(S_, 1, slru_o, sconv_o)
    try:
        program()
    except _Stop:
        pass
    for d in (t_out.lw,):
        for sem, val in d.values():
            emit_wait(POOL, sem, val)
    if kstop is not None:
        for sem, val in all_dma.values():
            emit_wait(POOL, sem, val)
        for E in (PE, ACT, DVE, SP):
            if E.cnt:
                emit_wait(POOL, E.sem, E.cnt)
    es.close()
    return nc


def _perm_w_in(w_in, cfg):
    D = cfg.D
    KC = D // 128
    o_u = 512 + 512 + 64
    cols = list(range(0, o_u)) + [0] * 64
    pad_mask = np.ones(len(cols), dtype=bool)
    pad_mask[o_u:] = False
    for j in range(KC):
        for g in range(4):
            base = o_u + g * D + j * 128
            cols += list(range(base, base + 128))
    cols = np.asarray(cols)
    out = np.ascontiguousarray(w_in[:, :, cols])
    out[:, :, o_u:o_u + 64] = 0.0
    return out


def _interleave13(w1, w3):
    sh = w1.shape
    F_ = sh[-1]
    a = w1.reshape(sh[:-1] + (F_ // 128, 1, 128))
    b = w3.reshape(sh[:-1] + (F_ // 128, 1, 128))
    return np.ascontiguousarray(np.concatenate([a, b], axis=-2).reshape(sh[:-1] + (2 * F_,)))


def _pack_vecs(inp, cfg):
    KC = cfg.D // 128
    out = np.zeros((128, 2 * NV), np.float32)

    def put(l, name, arr):
        n = arr.size // 128
        out[:, l * NV + VO[name]:l * NV + VO[name] + n] = arr.reshape(n, 128).T
    for l in range(2):
        put(l, "qg", inp["q_norm_g"][l])
        put(l, "kvg", inp["kv_norm_g"][l])
        put(l, "cw", inp["conv_w"][l].reshape(-1))
        put(l, "cb", inp["conv_b"][l])
        put(l, "ba", inp["b_gate_a"][l].reshape(-1))
        put(l, "bx", inp["b_gate_x"][l].reshape(-1))
        put(l, "lam", inp["lru_lambda"][l])
        put(l, "l1g", inp["ln1_g"][l])
        put(l, "l1b", inp["ln1_b"][l])
        put(l, "l2g", inp["ln2_g"][l])
        put(l, "l2b", inp["ln2_b"][l])
    return out


def _rope_table(pos):
    half = 32
    inv = (10000.0 ** (-np.arange(half, dtype=np.float32) / half)).astype(np.float32)
    ang = pos.astype(np.float32)[None, :] * inv[:, None]
    cos = np.cos(ang).astype(np.float32)
    sin = np.sin(ang).astype(np.float32)
    tab = np.zeros((64, 2, pos.size), np.float32)
    tab[0:32, 0], tab[32:64, 0] = cos, cos
    tab[0:32, 1], tab[32:64, 1] = sin, sin
    return tab


_NC_CACHE = {}


def run(inp, cfg):
    inp = {k: np.asarray(v) for k, v in inp.items()}
    D, S, TW, NCORES = cfg.D, cfg.S, cfg.TW, cfg.NCORES
    KC = D // 128
    NT = S // TW
    NH1 = NT // 2
    HS = S // 2
    key = (cfg.S, cfg.DFF, cfg.DFFE, cfg.PAST, cfg.NCORES)
    if key not in _NC_CACHE:
        _NC_CACHE[key] = build(cfg)
    nc = _NC_CACHE[key]
    shared = {
        "w_in": _perm_w_in(inp["w_in"], cfg),
        "w_uq": inp["w_uq"], "w_uk": inp["w_uk"], "w_uv": inp["w_uv"],
        "w_ga": np.ascontiguousarray(inp["w_gate_a"].reshape(2, KC * 128, 128)),
        "w_gx": np.ascontiguousarray(inp["w_gate_x"].reshape(2, KC * 128, 128)),
        "w_o": inp["w_o"],
        "w13": _interleave13(inp["ffn_w1"][0], inp["ffn_w3"][0]),
        "w2": inp["ffn_w2"][0],
        "wr": inp["router_w"][0],
        "m13": _interleave13(inp["moe_w1"][0], inp["moe_w3"][0]),
        "m2": inp["moe_w2"][0],
        "vecs": _pack_vecs(inp, cfg),
        "ident": np.eye(128, dtype=np.float32),
        "ropes": _rope_table(cfg.PAST + np.arange(cfg.DS)),
    }
    rm = np.zeros((64, 64), np.float32)
    for m in range(32):
        rm[m + 32, m] = -1.0
        rm[m, m + 32] = 1.0
    shared["rm"] = rm
    selm = np.zeros((cfg.NE, cfg.NE, 128), np.float32)
    for e in range(cfg.NE):
        selm[e, e, :] = 1.0
    shared["sel"] = selm
    in_maps = []
    for c in range(NCORES):
        s, r = (c // 2) % cfg.NPAIR, c % 2
        b = c % cfg.NSAMP
        m = dict(shared)
        xs = inp["x_prompt"][s]
        tmask = np.ones((128, NT), np.float32)
        kbias = np.zeros((128, 4 * NT), np.float32)
        if r == 0:
            x = np.concatenate([np.zeros((HS, D), np.float32), xs[:HS]], 0)
            pos = np.concatenate([np.zeros(HS), np.arange(HS)]).astype(np.float32)
            tmask[:, :NH1] = 0.0
            kbias[:, :4 * NH1] = -60.0
        else:
            x = xs
            pos = np.arange(S).astype(np.float32)
        m["x"] = np.ascontiguousarray(x)
        m["rope"] = _rope_table(pos)
        m["tmask"] = tmask
        m["kbias"] = kbias
        m["xs"] = np.ascontiguousarray(inp["x_sample"][b])
        m["clat"] = np.ascontiguousarray(inp["cache_kv_latent"][:, b])
        m["ckr"] = np.ascontiguousarray(inp["cache_k_rope"][:, b])
        m["slru"] = np.ascontiguousarray(inp["state_lru"][:, b].reshape(2, KC, 128).transpose(0, 2, 1))
        m["sconv"] = np.ascontiguousarray(inp["state_conv"][:, b].reshape(2, 3, KC, 128).transpose(0, 3, 2, 1))
        in_maps.append(m)
    res = run_bass_kernel_spmd(nc, in_maps, core_ids=list(range(NCORES)))
    R = res.results
    NP, NS = cfg.NPAIR, cfg.NSAMP
    y = np.stack([np.concatenate([R[2 * s]["y"], R[2 * s + 1]["y"]], 0) for s in range(NP)])
    ys = np.stack([R[b]["ys"] for b in range(NS)])
    p_lat = np.stack([R[2 * s + 1]["lat_o"] for s in range(NP)], 1)
    p_kr = np.stack([R[2 * s + 1]["kr_o"] for s in range(NP)], 1)

    def unlru(a):
        return np.ascontiguousarray(a.transpose(0, 2, 1).reshape(2, D))

    def unconv(a):
        return np.ascontiguousarray(a.transpose(0, 3, 2, 1).reshape(2, 3, D))
    p_lru = np.stack([unlru(R[2 * s + 1]["lru_o"]) for s in range(NP)], 1)
    p_conv = np.stack([unconv(R[2 * s + 1]["conv_o"]) for s in range(NP)], 1)
    s_lat = np.stack([R[b]["slat_o"] for b in range(NS)], 1)
    s_kr = np.stack([R[b]["skr_o"] for b in range(NS)], 1)
    s_lru = np.stack([unlru(R[b]["slru_o"]) for b in range(NS)], 1)
    s_conv = np.stack([unconv(R[b]["sconv_o"]) for b in range(NS)], 1)
    return tuple(np.ascontiguousarray(a.astype(np.float32)) for a in
                 (y, ys, p_lat, p_kr, p_lru, p_conv, s_lat, s_kr, s_lru, s_conv))


def kernel(**inputs):
    return run(inputs, Cfg)
```
